# Optimizing a Trainium2 kernel written in Bass

```python
import jax, jax.numpy as jnp
from jax import lax
import numpy as np


D_MODEL = 1024
BATCH = 2
SEQ = 16384
DEPTH = 1

A_WIDTH = D_MODEL // 2
A_HEAD = 64
A_HEADS = A_WIDTH // A_HEAD
A_DECAY_RANK = 64
A_ICLR_RANK = 64
A_GATE_RANK = 128
GN_EPS = 64e-5
B_WIDTH = D_MODEL // 2
B_BLOCKS = 8
B_BLOCK = B_WIDTH // B_BLOCKS
CONV_WIDTH = 4
LRU_C = 8.0
IN_COLS = 3 * A_WIDTH + 2 * B_WIDTH + 2 * D_MODEL
N_GROUPS = 4
EXPERTS_PER_GROUP = 8
N_EXPERTS = N_GROUPS * EXPERTS_PER_GROUP
TOP_K = 2
D_EXPERT = 256
EPS = 1e-6

kernel_name = 'hybrid_rwkv7_rglru_hmoe_adaln'


def rmsnorm(x, g):
    xf = x.astype(jnp.float32)
    y = xf * lax.rsqrt(jnp.mean(xf * xf, axis=-1, keepdims=True) + EPS)
    return (y * g.astype(jnp.float32)).astype(x.dtype)


def modulate(h, shift, scale):
    return h * (1 + scale[:, None, :]) + shift[:, None, :]


def token_shift(t):
    return jnp.pad(t, ((0, 0), (1, 0), (0, 0)))[:, :-1]


def rwkv7_time_mix(xn, rkv, mu_rkv, mu_wag, w0, w1, w2, a0, a1, a2, g1, g2,
                   k_k, k_a, r_k, lnx_g, lnx_b):
    bsz, seq, _ = xn.shape
    f32 = jnp.float32
    rkv = rkv + (token_shift(rkv) - rkv) * mu_rkv
    r, k, v = jnp.split(rkv, 3, axis=-1)
    dx = token_shift(xn) - xn
    xw = xn + dx * mu_wag[0]
    xa = xn + dx * mu_wag[1]
    xg = xn + dx * mu_wag[2]
    w_log = -jax.nn.softplus(-(w0 + jnp.tanh(xw @ w1) @ w2)) - 0.5
    decay = jnp.exp(-jnp.exp(w_log.astype(f32)))
    a = jax.nn.sigmoid(a0 + (xa @ a1) @ a2)
    g = jax.nn.sigmoid(xg @ g1) @ g2
    heads = lambda t: t.reshape(bsz, seq, A_HEADS, A_HEAD)
    kk = heads((k * k_k).astype(f32))
    kk = kk * lax.rsqrt(jnp.sum(kk * kk, axis=-1, keepdims=True) + 1e-12)
    k = k * (1 + (a - 1) * k_a)
    r_h, k_h, v_h, a_h = (heads(t.astype(f32)) for t in (r, k, v, a))
    w_h = heads(decay)
    seq_first = lambda t: jnp.moveaxis(t, 1, 0)
    xs = tuple(seq_first(t) for t in (r_h, w_h, k_h, v_h, -kk, kk * a_h))

    def step(state, inp):
        r_t, w_t, k_t, v_t, a_t, b_t = inp
        sa = jnp.einsum('bhvk,bhk->bhv', state, a_t)
        state = (state * w_t[:, :, None, :] + sa[..., None] * b_t[:, :, None, :]
                 + v_t[..., None] * k_t[:, :, None, :])
        y_t = jnp.einsum('bhvk,bhk->bhv', state, r_t)
        return state, y_t

    state0 = jnp.zeros((bsz, A_HEADS, A_HEAD, A_HEAD), f32)
    _, y = lax.scan(step, state0, xs)
    y = jnp.moveaxis(y, 0, 1)
    mean = jnp.mean(y, axis=-1, keepdims=True)
    var = jnp.mean(jnp.square(y - mean), axis=-1, keepdims=True)
    y = ((y - mean) * lax.rsqrt(var + GN_EPS)).reshape(bsz, seq, A_WIDTH)
    y = y * lnx_g.astype(f32) + lnx_b.astype(f32)
    bonus = jnp.sum(r_h * k_h * r_k.astype(f32), axis=-1, keepdims=True) * v_h
    y = y + bonus.reshape(bsz, seq, A_WIDTH)
    return (y * g.astype(f32)).astype(xn.dtype)


def rglru_branch(xb, gb, conv_w, conv_b, w_rgate, b_rgate, w_igate, b_igate, lam):
    bsz, seq, _ = xb.shape
    f32 = jnp.float32
    xc = lax.conv_general_dilated(
        xb, conv_w, window_strides=(1,), padding=((CONV_WIDTH - 1, 0),),
        dimension_numbers=('NWC', 'WIO', 'NWC'), feature_group_count=B_WIDTH) + conv_b
    xblk = xc.reshape(bsz, seq, B_BLOCKS, B_BLOCK)
    gate_r = jax.nn.sigmoid((jnp.einsum('bsgi,gij->bsgj', xblk, w_rgate)
                             .reshape(bsz, seq, B_WIDTH) + b_rgate).astype(f32))
    gate_i = jax.nn.sigmoid((jnp.einsum('bsgi,gij->bsgj', xblk, w_igate)
                             .reshape(bsz, seq, B_WIDTH) + b_igate).astype(f32))
    log_a = -LRU_C * gate_r * jax.nn.softplus(-lam.astype(f32))
    a = jnp.exp(log_a)
    u = jnp.sqrt(-jnp.expm1(2.0 * log_a)) * (gate_i * xc.astype(f32))

    def combine(left, right):
        a_l, u_l = left
        a_r, u_r = right
        return a_l * a_r, a_r * u_l + u_r

    _, h = lax.associative_scan(combine, (a, u), axis=1)
    return (h * jax.nn.gelu(gb.astype(f32))).astype(xb.dtype)


def hier_moe(h, w_rg, b_rg, w_re, b_re, w1e, w3e, w2e):
    bsz, seq, d = h.shape
    f32 = jnp.float32
    t = h.reshape(-1, d)
    pg = jax.nn.softmax((t @ w_rg + b_rg).astype(f32), axis=-1)
    pg_top, g_idx = lax.top_k(pg, 1)
    le = (t @ w_re + b_re).astype(f32).reshape(-1, N_GROUPS, EXPERTS_PER_GROUP)
    le_sel = jnp.take_along_axis(le, g_idx[:, :, None], axis=1)[:, 0]
    pe = jax.nn.softmax(le_sel, axis=-1)
    pe_top, e_idx = lax.top_k(pe, TOP_K)
    pe_top = pe_top / jnp.sum(pe_top, axis=-1, keepdims=True)
    wts = pg_top * pe_top
    flat_idx = g_idx * EXPERTS_PER_GROUP + e_idx
    cw = jnp.sum(jax.nn.one_hot(flat_idx, N_EXPERTS, dtype=f32) * wts[..., None], axis=1)
    cw = cw.astype(h.dtype)
    out = jnp.zeros_like(t)
    for e in range(N_EXPERTS):
        he = jax.nn.silu(t @ w1e[e]) * (t @ w3e[e])
        out = out + cw[:, e:e + 1] * (he @ w2e[e])
    return out.reshape(bsz, seq, d)


def setup_inputs(seed: int = 0) -> dict:
    key = jax.random.key(seed)
    ks = iter(jax.random.split(key, 64))
    nrm = lambda shape, s: jax.random.normal(next(ks), shape, jnp.float32) * s
    uni = lambda shape, lo, hi: jax.random.uniform(next(ks), shape, jnp.float32, lo, hi)
    L, D, A, Bw, F = DEPTH, D_MODEL, A_WIDTH, B_WIDTH, D_EXPERT
    x = nrm((BATCH, SEQ, D), 1.0)
    c = nrm((BATCH, D), 1.0)
    w_ada = nrm((L, D, 6 * D), D ** -0.5)
    b_ada = nrm((L, 6 * D), 0.02)
    g_mix = 1.0 + nrm((L, D), 0.02)
    w_in = nrm((L, D, IN_COLS), D ** -0.5)
    mu_rkv = uni((L, 3 * A), 0.0, 1.0)
    mu_wag = uni((L, 3, D), 0.0, 1.0)
    w0 = uni((L, A), -6.0, -1.0)
    w1 = nrm((L, D, A_DECAY_RANK), D ** -0.5)
    w2 = nrm((L, A_DECAY_RANK, A), 0.5 * A_DECAY_RANK ** -0.5)
    a0 = nrm((L, A), 0.1)
    a1 = nrm((L, D, A_ICLR_RANK), D ** -0.5)
    a2 = nrm((L, A_ICLR_RANK, A), 0.5 * A_ICLR_RANK ** -0.5)
    g1 = nrm((L, D, A_GATE_RANK), D ** -0.5)
    g2 = nrm((L, A_GATE_RANK, A), A_GATE_RANK ** -0.5)
    k_k = 0.85 + nrm((L, A), 0.02)
    k_a = 1.0 + nrm((L, A), 0.02)
    r_k = nrm((L, A_HEADS, A_HEAD), 0.1)
    lnx_g = 1.0 + nrm((L, A), 0.02)
    lnx_b = nrm((L, A), 0.02)
    conv_w = nrm((L, CONV_WIDTH, 1, Bw), CONV_WIDTH ** -0.5)
    conv_b = nrm((L, Bw), 0.02)
    w_rgate = nrm((L, B_BLOCKS, B_BLOCK, B_BLOCK), B_BLOCK ** -0.5)
    b_rgate = nrm((L, Bw), 0.02)
    w_igate = nrm((L, B_BLOCKS, B_BLOCK, B_BLOCK), B_BLOCK ** -0.5)
    b_igate = nrm((L, Bw), 0.02)
    s = uni((L, Bw), 0.9, 0.999) ** (1.0 / LRU_C)
    lam = jnp.log(s) - jnp.log1p(-s)
    p_a = nrm((L, A, D), A ** -0.5)
    p_b = nrm((L, Bw, D), Bw ** -0.5)
    w_out = nrm((L, D, D), D ** -0.5)
    g_ffn = 1.0 + nrm((L, D), 0.02)
    w_rg = nrm((L, D, N_GROUPS), D ** -0.5)
    b_rg = nrm((L, N_GROUPS), 0.01)
    w_re = nrm((L, D, N_EXPERTS), D ** -0.5)
    b_re = nrm((L, N_EXPERTS), 0.01)
    w1e = nrm((L, N_EXPERTS, D, F), D ** -0.5)
    w3e = nrm((L, N_EXPERTS, D, F), D ** -0.5)
    w2e = nrm((L, N_EXPERTS, F, D), F ** -0.5)
    g_final = 1.0 + nrm((D,), 0.02)
    w_ada_f = nrm((D, 2 * D), D ** -0.5)
    b_ada_f = nrm((2 * D,), 0.02)
    return {'x': x, 'c': c, 'w_ada': w_ada, 'b_ada': b_ada, 'g_mix': g_mix, 'w_in': w_in,
            'mu_rkv': mu_rkv, 'mu_wag': mu_wag, 'w0': w0, 'w1': w1, 'w2': w2,
            'a0': a0, 'a1': a1, 'a2': a2, 'g1': g1, 'g2': g2, 'k_k': k_k, 'k_a': k_a,
            'r_k': r_k, 'lnx_g': lnx_g, 'lnx_b': lnx_b, 'conv_w': conv_w, 'conv_b': conv_b,
            'w_rgate': w_rgate, 'b_rgate': b_rgate, 'w_igate': w_igate, 'b_igate': b_igate,
            'lam': lam, 'p_a': p_a, 'p_b': p_b, 'w_out': w_out, 'g_ffn': g_ffn,
            'w_rg': w_rg, 'b_rg': b_rg, 'w_re': w_re, 'b_re': b_re,
            'w1e': w1e, 'w3e': w3e, 'w2e': w2e,
            'g_final': g_final, 'w_ada_f': w_ada_f, 'b_ada_f': b_ada_f}


def reference(x, c, w_ada, b_ada, g_mix, w_in, mu_rkv, mu_wag, w0, w1, w2, a0, a1, a2,
              g1, g2, k_k, k_a, r_k, lnx_g, lnx_b, conv_w, conv_b, w_rgate, b_rgate,
              w_igate, b_igate, lam, p_a, p_b, w_out, g_ffn, w_rg, b_rg, w_re, b_re,
              w1e, w3e, w2e, g_final, w_ada_f, b_ada_f):
    c_act = jax.nn.silu(c)
    o1 = 3 * A_WIDTH
    o2 = o1 + B_WIDTH
    o3 = o2 + B_WIDTH
    o4 = o3 + D_MODEL
    for l in range(DEPTH):
        mod = c_act @ w_ada[l] + b_ada[l]
        sh1, sc1, gt1, sh2, sc2, gt2 = jnp.split(mod, 6, axis=-1)
        h = modulate(rmsnorm(x, g_mix[l]), sh1, sc1)
        proj = h @ w_in[l]
        y_a = rwkv7_time_mix(h, proj[..., :o1], mu_rkv[l], mu_wag[l], w0[l], w1[l], w2[l],
                             a0[l], a1[l], a2[l], g1[l], g2[l], k_k[l], k_a[l], r_k[l],
                             lnx_g[l], lnx_b[l])
        y_b = rglru_branch(proj[..., o1:o2], proj[..., o2:o3], conv_w[l], conv_b[l],
                           w_rgate[l], b_rgate[l], w_igate[l], b_igate[l], lam[l])
        merged = (jax.nn.sigmoid(proj[..., o3:o4]) * (y_a @ p_a[l])
                  + jax.nn.sigmoid(proj[..., o4:]) * (y_b @ p_b[l]))
        x = x + gt1[:, None, :] * (merged @ w_out[l])
        h2 = modulate(rmsnorm(x, g_ffn[l]), sh2, sc2)
        x = x + gt2[:, None, :] * hier_moe(h2, w_rg[l], b_rg[l], w_re[l], b_re[l],
                                           w1e[l], w3e[l], w2e[l])
    shf, scf = jnp.split(c_act @ w_ada_f + b_ada_f, 2, axis=-1)
    return modulate(rmsnorm(x, g_final), shf, scf)
```

```python
import numpy as np
from contextlib import ExitStack
import concourse.bass as bass
import concourse.mybir as mybir
from concourse.bass_utils import run_bass_kernel_spmd

F32, BF16 = mybir.dt.float32, mybir.dt.bfloat16
AF = mybir.ActivationFunctionType
ALU = mybir.AluOpType
AX = mybir.AxisListType
ENGS = ["pe", "act", "dve", "pool", "sp"]
D = 1024
SEQ = 16384
TT = 512
CH = 64
NCH = TT // CH
EPS = 1e-6
GN_EPS = 64e-5


class Prog:
    def __init__(self, nc, es):
        self.nc, self.es = nc, es
        self.ops = {e: [] for e in ENGS}
        self.lastw = {}
        self.readers = {}
        self.dsem = {}
        self.same_engine_sync = True
        self.nps = 0

    def sb(self, name, shape, dt):
        return self.es.enter_context(self.nc.sbuf_tensor(name, list(shape), dt))

    def ps(self, name, shape, dt):
        return self.es.enter_context(self.nc.psum_tensor(name, list(shape), dt))

    def _deps(self, reads, writes):
        deps = set()
        for k in reads:
            if k in self.lastw:
                deps.add(self.lastw[k])
        for k in writes:
            if k in self.lastw:
                deps.add(self.lastw[k])
            for kk, v in self.readers.get(k, {}).items():
                deps.add((kk[0], kk[1], v))
        return deps

    def _commit(self, tok, reads, writes):
        for k in reads:
            r = self.readers.setdefault(k, {})
            kk = (tok[0], tok[1])
            r[kk] = max(r.get(kk, -1), tok[2])
        for k in writes:
            self.lastw[k] = tok
            self.readers[k] = {}

    def op(self, eng, fn, reads=(), writes=(), inc=True):
        deps = self._deps(reads, writes)
        tok = ("E", eng, len(self.ops[eng]))
        self.ops[eng].append(dict(fn=fn, deps=deps, inc=inc, dma=None))
        self._commit(tok, reads, writes)
        return tok

    def dma(self, eng, out, in_, reads=(), writes=(), semkey=None):
        deps = self._deps(reads, writes)
        n = self.dsem.get(semkey, 0) + 1
        self.dsem[semkey] = n
        if n > 1:
            deps.add(("D", semkey, 16 * (n - 1)))
        tok = ("D", semkey, 16 * n)
        self.ops[eng].append(dict(fn=lambda e: e.dma_start(out=out, in_=in_), deps=deps, inc=False, dma=semkey))
        self._commit(tok, reads, writes)
        return tok

    def wait_all(self, eng, toks):
        self.ops[eng].append(dict(fn=None, deps=set(toks), inc=False, dma=None))

    def emit(self):
        nc = self.nc
        sems = {e: self.es.enter_context(nc.semaphore("s_" + e)) for e in ENGS}
        dsems = {k: self.es.enter_context(nc.semaphore("d%d" % i)) for i, k in enumerate(self.dsem)}
        need = {}
        for e in ENGS:
            c = 0
            arr = []
            for o in self.ops[e]:
                if o["inc"]:
                    c += 1
                arr.append(c)
            nd = [None] * len(arr)
            nxt = None
            for i in range(len(arr) - 1, -1, -1):
                if self.ops[e][i]["inc"]:
                    nxt = arr[i]
                nd[i] = nxt
            need[e] = nd
        block = self.es.enter_context(nc.Block())

        def run(e, eng):
            waited = {}
            for o in self.ops[e]:
                for d in sorted(o["deps"], key=str):
                    if d[0] == "E":
                        _, e2, j = d
                        if e2 == e and (e == "pe" or not self.same_engine_sync):
                            continue
                        v = need[e2][j]
                        assert v is not None, (e2, j)
                        s = sems[e2]
                        key = "E" + e2
                    else:
                        _, k, v = d
                        s = dsems[k]
                        key = "D" + str(k)
                    if waited.get(key, 0) >= v:
                        continue
                    waited[key] = v
                    eng.wait_ge(s, v)
                if o["fn"] is None:
                    continue
                ins = o["fn"](eng)
                if o["dma"] is not None:
                    ins.then_inc(dsems[o["dma"]], 16)
                elif o["inc"]:
                    ins.then_inc(sems[e], 1)

        block.tensor(lambda eng: run("pe", eng))
        block.scalar(lambda eng: run("act", eng))
        block.vector(lambda eng: run("dve", eng))
        block.gpsimd(lambda eng: run("pool", eng))
        block.sync(lambda eng: run("sp", eng))


def _mm(P, out, lhsT, rhs, start, stop, reads, writes):
    P.op("pe", lambda e: e.matmul(out, lhsT, rhs, start=start, stop=stop), reads, writes, inc=stop)


def _tr(P, out, in_, ident, reads, writes, inc=True):
    P.op("pe", lambda e: e.transpose(out, in_, ident), reads, writes, inc=inc)


def _act(P, out, in_, func, reads, writes, bias=0.0, scale=1.0, accum=None, eng="act"):
    if accum is None:
        P.op(eng, lambda e: e.activation(out=out, in_=in_, func=func, bias=bias, scale=scale), reads, writes)
    else:
        P.op(eng, lambda e: e.activation(out=out, in_=in_, func=func, bias=bias, scale=scale, accum_out=accum),
             reads, writes)


def _ts(P, eng, out, in0, s1, s2, op0, op1, reads, writes):
    if s2 is None:
        P.op(eng, lambda e: e.tensor_scalar(out=out, in0=in0, scalar1=s1, scalar2=None, op0=op0), reads, writes)
    else:
        P.op(eng, lambda e: e.tensor_scalar(out=out, in0=in0, scalar1=s1, scalar2=s2, op0=op0, op1=op1), reads, writes)


def _stt(P, eng, out, in0, sc, in1, op0, op1, reads, writes):
    P.op(eng, lambda e: e.scalar_tensor_tensor(out=out, in0=in0, scalar=sc, in1=in1, op0=op0, op1=op1), reads, writes)


def _tt(P, eng, out, in0, in1, op, reads, writes):
    P.op(eng, lambda e: e.tensor_tensor(out=out, in0=in0, in1=in1, op=op), reads, writes)


def _cp(P, eng, out, in_, reads, writes):
    if eng == "act":
        P.op(eng, lambda e: e.copy(out=out, in_=in_), reads, writes)
    else:
        P.op(eng, lambda e: e.tensor_copy(out=out, in_=in_), reads, writes)


V_MUR, V_MUK, V_MUV, V_W0, V_A0, V_KK, V_KA, V_RK, V_LG, V_LB, V_CW0, V_CW1, V_CW2, V_CW3, V_CB, V_BR, V_BI, V_LAM = range(18)
NV = 18


def build_l1(seq):
    nt = seq // TT
    nc = bass.Bass("TRN2", target_bir_lowering=False)
    dr = lambda n, s, k="ExternalInput", dt=F32: nc.dram_tensor(n, list(s), dt, kind=k).ap()
    x = dr("x", [seq, D])
    cT = dr("cT", [128, 8])
    wada = dr("wada", [D, 2048])
    bada = dr("bada", [1, 2048])
    gmix = dr("gmix", [1, D])
    wrkv = dr("wrkv", [D, 384])
    wxg = dr("wxg", [D, 256])
    w1a1 = dr("w1a1", [D, 128])
    g1 = dr("g1", [D, 128])
    muT = dr("muT", [128, 24])
    w2a2 = dr("w2a2", [128, 128])
    g2o = dr("g2o", [128, 128])
    vecs = dr("vecs", [128, NV])
    wrg = dr("wrg", [128, 64])
    wig = dr("wig", [128, 64])
    cid = dr("cid", [128, 128])
    cbo = dr("cbo", [128, 128])
    cmsk = dr("cmsk", [128, 12 * 512])
    crst = dr("crst", [128, 512])
    ya_o = dr("ya", [128, seq], "ExternalOutput")
    yb_o = dr("yb", [128, seq], "ExternalOutput")

    es = ExitStack()
    with es:
        P = Prog(nc, es)
        sb, psum = P.sb, P.ps
        ident = sb("ident", [128, 128], BF16)
        bones = sb("bones", [128, 128], BF16)
        msk = sb("msk", [128, 12, 512], BF16)
        rstm = sb("rstm", [128, 512], F32)
        Wrkv = sb("Wrkv", [128, 8, 384], BF16)
        Wxg = sb("Wxg", [128, 8, 256], BF16)
        W1f = sb("W1f", [128, 8, 128], F32)
        G1f = sb("G1f", [128, 8, 128], F32)
        W1a = sb("W1a", [128, 8, 128], BF16)
        W1b = sb("W1b", [128, 8, 128], BF16)
        G1a = sb("G1a", [128, 8, 128], BF16)
        G1b = sb("G1b", [128, 8, 128], BF16)
        mu = sb("mu", [128, 24], F32)
        omu = sb("omu", [128, 24], F32)
        W2A2 = sb("W2A2", [128, 128], BF16)
        G2 = sb("G2", [128, 128], BF16)
        vc = sb("vc", [128, NV], F32)
        Wrg = sb("Wrg", [128, 64], BF16)
        Wig = sb("Wig", [128, 64], BF16)
        cTs = sb("cTs", [128, 8], F32)
        cact = sb("cact", [128, 8], F32)
        cbc = sb("cbc", [128, 8, 128], F32)
        wad = sb("wad", [128, 8, 256], F32)
        modb = sb("modb", [128, 2048], F32)
        badr = sb("badr", [1, 2048], F32)
        ones1 = sb("ones1", [1, 128], F32)
        gsc = sb("gsc", [128, D], F32)
        m8 = sb("m8", [128, 2], F32)
        tmpc = sb("tmpc", [128, 2], F32)

        pool_banks = [psum("pb%d" % i, [128, 512], F32) for i in range(6)]
        pch = psum("pch", [128, 512], F32)
        pyt = psum("pyt", [128, 512], F32)

        def bank():
            i = P.nps % 6
            P.nps += 1
            return pool_banks[i], ("pb", i)

        ld = lambda out, in_, key, eng="sp": P.dma(eng, out, in_, [], [key], semkey=key)
        ld(ident[:], cid[:, :], "ident", "pool")
        ld(bones[:], cbo[:, :], "bones", "pool")
        ld(msk[:], cmsk.rearrange("p (a t) -> p a t", a=12), "msk", "pool")
        ld(rstm[:], crst[:, :], "rstm")
        ld(Wrkv[:], wrkv.rearrange("(k p) c -> p k c", p=128), "Wrkv", "pool")
        ld(Wxg[:], wxg.rearrange("(k p) c -> p k c", p=128), "Wxg", "pool")
        ld(W1f[:], w1a1.rearrange("(k p) c -> p k c", p=128), "W1f")
        ld(G1f[:], g1.rearrange("(k p) c -> p k c", p=128), "G1f")
        ld(mu[:], muT[:, :], "mu")
        ld(W2A2[:], w2a2[:, :], "W2A2", "pool")
        ld(G2[:], g2o[:, :], "G2", "pool")
        ld(vc[:], vecs[:, :], "vc")
        ld(Wrg[:], wrg[:, :], "Wrg", "pool")
        ld(Wig[:], wig[:, :], "Wig", "pool")
        ld(cTs[:], cT[:, :], "cTs")
        ld(badr[:], bada[:, :], "badr")
        ld(gsc[:], gmix.partition_broadcast(128), "gsc")
        P.op("dve", lambda e: e.memset(ones1[:], 1.0), [], ["ones1"])
        _ts(P, "dve", omu[:], mu[:], -1.0, 1.0, ALU.mult, ALU.add, ["mu"], ["omu"])
        for k in range(8):
            _ts(P, "dve", W1a[:, k, 0:64], W1f[:, k, 0:64], omu[:, k:k + 1], None, ALU.mult, None, ["W1f", "omu"], ["W1a"])
            _ts(P, "dve", W1b[:, k, 0:64], W1f[:, k, 0:64], mu[:, k:k + 1], None, ALU.mult, None, ["W1f", "mu"], ["W1b"])
            _ts(P, "dve", W1a[:, k, 64:128], W1f[:, k, 64:128], omu[:, 8 + k:9 + k], None, ALU.mult, None, ["W1f", "omu"], ["W1a"])
            _ts(P, "dve", W1b[:, k, 64:128], W1f[:, k, 64:128], mu[:, 8 + k:9 + k], None, ALU.mult, None, ["W1f", "mu"], ["W1b"])
            _ts(P, "dve", G1a[:, k, :], G1f[:, k, :], omu[:, 16 + k:17 + k], None, ALU.mult, None, ["G1f", "omu"], ["G1a"])
            _ts(P, "dve", G1b[:, k, :], G1f[:, k, :], mu[:, 16 + k:17 + k], None, ALU.mult, None, ["G1f", "mu"], ["G1b"])
        _act(P, cact[:], cTs[:], AF.Silu, ["cTs"], ["cact"])
        for k in range(8):
            _cp(P, "dve", cbc[:, k, :], cact[:, k:k + 1].to_broadcast([128, 128]), ["cact"], ["cbc"])
        for cc in range(8):
            ld(wad[:], wada[:, cc * 256:(cc + 1) * 256].rearrange("(k p) c -> p k c", p=128), "wad")
            bk, bkey = bank()
            for k in range(8):
                _mm(P, bk[:, 0:256], cbc[:, k, :], wad[:, k, :], k == 0, False, ["cbc", "wad"], [bkey])
            _mm(P, bk[:, 0:256], ones1[:, :], badr[:, cc * 256:(cc + 1) * 256], False, True, ["ones1", "badr"], [bkey])
            _cp(P, "dve", modb[:, cc * 256:(cc + 1) * 256], bk[:, 0:256], [bkey], ["modb"])
        _stt(P, "dve", gsc[:], modb[:, 1024:2048], 1.0, gsc[:], ALU.add, ALU.mult, ["modb", "gsc"], ["gsc"])
        _act(P, tmpc[:, 0:1], vc[:, V_LAM:V_LAM + 1], AF.Exp, ["vc"], ["tmpc"], scale=-1.0)
        _act(P, tmpc[:, 1:2], tmpc[:, 0:1], AF.Ln, ["tmpc"], ["tmpc"], bias=1.0)
        _ts(P, "dve", m8[:, 0:1], tmpc[:, 1:2], -8.0, None, ALU.mult, None, ["tmpc"], ["m8"])
        _ts(P, "dve", m8[:, 1:2], tmpc[:, 1:2], -16.0, None, ALU.mult, None, ["tmpc"], ["m8"])

        xt = sb("xt", [128, 4, D], F32)
        junk = sb("junk", [128, D], F32)
        ss = sb("ss", [128, 4], F32)
        rstd = sb("rstd", [128, 4], F32)
        hn = sb("hn", [128, 4, D], BF16)
        hT = sb("hT", [128, 8, TT + 1], BF16)
        R3 = [sb("R3_%d" % i, [128, TT + 1], F32) for i in range(3)]
        XB = sb("XB", [128, TT + 3], F32)
        GB = sb("GB", [128, TT], F32)
        L1 = sb("L1", [128, TT], BF16)
        LG = sb("LG", [128, TT], BF16)
        f32t = lambda n: sb(n, [128, TT], F32)
        bft = lambda n: sb(n, [128, TT], BF16)
        sg, ai, gS = f32t("sg"), f32t("ai"), f32t("gS")
        rm, km, vm = f32t("rm"), f32t("km"), f32t("vm")
        kk, kkn, kmod, t1 = f32t("kk"), f32t("kkn"), f32t("kmod"), f32t("t1")
        kk2 = bft("kk2")
        rn = f32t("rn")
        av, bv = f32t("av"), f32t("bv")
        lw, cc_, E1, E2, E3, E4, d4 = f32t("lw"), f32t("cc"), f32t("E1"), f32t("E2"), f32t("E3"), f32t("E4"), f32t("d4")
        dd = d4
        gC = sb("gC", [128, NCH], F32)
        AR = sb("AR", [128, NCH, 2, CH], BF16)
        BK = sb("BK", [128, NCH, 2, CH], BF16)
        bh, kh, vb = bft("bh"), bft("kh"), bft("vb")
        Bt = sb("Bt", [128, NCH, CH], BF16)
        Kt = sb("Kt", [128, NCH, CH], BF16)
        Vt = sb("Vt", [128, NCH, CH], BF16)
        NTA = sb("NTA", [128, NCH, 2, CH], BF16)
        AKK = sb("AKK", [128, NCH, 2, CH], BF16)
        Ap = [sb("Ap%d" % i, [128, NCH, CH], BF16) for i in range(2)]
        Xf = sb("Xf", [128, NCH, CH], F32)
        Yf = sb("Yf", [128, NCH, CH], F32)
        Xb = sb("Xb", [128, NCH, CH], BF16)
        Yb = sb("Yb", [128, NCH, CH], BF16)
        H = sb("H", [128, CH], F32)
        Hb = sb("Hb", [128, CH], BF16)
        Wb = sb("Wb", [128, CH], BF16)
        Ub = sb("Ub", [128, CH], BF16)
        yT, dY, gn, bon, yo = f32t("yT"), f32t("dY"), f32t("gn"), f32t("bon"), f32t("yo")
        yTb, d2b, rkb = bft("yTb"), bft("d2b"), bft("rkb")
        xc, gr, gi, aa, a2, sq, uu, hh_, ge, ybo, xcb = kk, kkn, t1, E1, E2, E3, E4, d4, lw, cc_, kk2
        hcar = sb("hcar", [128, 1], F32)

        P.op("dve", lambda e: e.memset(hT[:], 0.0), [], ["hT"])
        for i in range(3):
            P.op("pool", lambda e, i=i: e.memset(R3[i][:], 0.0), [], ["R3_%d" % i])
        P.op("pool", lambda e: e.memset(XB[:], 0.0), [], ["XB"])
        P.op("dve", lambda e: e.memset(H[:], 0.0), [], ["H"])
        P.op("dve", lambda e: e.memset(Hb[:], 0.0), [], ["Hb"])
        P.op("dve", lambda e: e.memset(hcar[:], 0.0), [], ["hcar"])

        v = lambda i: vc[:, i:i + 1]
        r3 = lambda t: t.rearrange("p (j t) -> p j t", t=CH)
        out_toks = []
        for it in range(nt):
            t0 = it * TT
            P.dma("sp", xt[:], x[t0:t0 + TT, :].rearrange("(s p) d -> p s d", p=128), [], ["xt"], semkey="xt")
            for s in range(4):
                _act(P, junk[:], xt[:, s, :], AF.Square, ["xt"], ["junk", "ss"], accum=ss[:, s:s + 1])
            _act(P, rstd[:], ss[:], AF.Sqrt, ["ss"], ["rstd"], bias=EPS, scale=1.0 / D)
            P.op("dve", lambda e: e.reciprocal(out=rstd[:], in_=rstd[:]), ["rstd"], ["rstd"])
            for s in range(4):
                _stt(P, "dve", junk[:], xt[:, s, :], rstd[:, s:s + 1], gsc[:], ALU.mult, ALU.mult,
                     ["xt", "rstd", "gsc"], ["junk"])
                _tt(P, "pool", hn[:, s, :], junk[:], modb[:, 0:1024], ALU.add, ["junk", "modb"], ["hn"])
            for s in range(4):
                bk, bkey = bank()
                bkb = bk[:, :].bitcast(BF16)
                for k in range(8):
                    _tr(P, bkb[:, k * 128:(k + 1) * 128], hn[:, s, k * 128:(k + 1) * 128], ident[:],
                        ["hn", "ident"], [bkey], inc=(k == 7))
                _cp(P, "act" if s % 2 else "dve", hT[:, :, 1 + 128 * s:1 + 128 * (s + 1)],
                    bkb[:, 0:1024].rearrange("p (k t) -> p k t", k=8), [bkey], ["hT"])
            def proj(W, wkey, c0, dst, dkey, ev="act"):
                bk, bkey = bank()
                for k in range(8):
                    _mm(P, bk[:, :], W[:, k, c0:c0 + 128], hT[:, k, 1:TT + 1], k == 0, k == 7, [wkey, "hT"], [bkey])
                _cp(P, ev, dst, bk[:, :], [bkey], [dkey])
            for i in range(3):
                proj(Wrkv, "Wrkv", 128 * i, R3[i][:, 1:TT + 1], "R3_%d" % i, "act" if i % 2 else "dve")
            proj(Wxg, "Wxg", 0, XB[:, 3:TT + 3], "XB", "act")
            proj(Wxg, "Wxg", 128, GB[:, :], "GB", "dve")
            bk, bkey = bank()
            for k in range(8):
                _mm(P, bk[:, :], W1a[:, k, :], hT[:, k, 1:TT + 1], k == 0, False, ["W1a", "hT"], [bkey])
            for k in range(8):
                _mm(P, bk[:, :], W1b[:, k, :], hT[:, k, 0:TT], False, k == 7, ["W1b", "hT"], [bkey])
            _act(P, L1[0:64, :], bk[0:64, :], AF.Tanh, [bkey], ["L1"])
            _cp(P, "dve", L1[64:128, :], bk[64:128, :], [bkey], ["L1"])
            bk, bkey = bank()
            for k in range(8):
                _mm(P, bk[:, :], G1a[:, k, :], hT[:, k, 1:TT + 1], k == 0, False, ["G1a", "hT"], [bkey])
            for k in range(8):
                _mm(P, bk[:, :], G1b[:, k, :], hT[:, k, 0:TT], False, k == 7, ["G1b", "hT"], [bkey])
            _act(P, LG[:, :], bk[:, :], AF.Sigmoid, [bkey], ["LG"])
            _cp(P, "pool", hT[:, :, 0:1], hT[:, :, TT:TT + 1], ["hT"], ["hT"])
            bk, bkey = bank()
            _mm(P, bk[:, :], W2A2[0:64, :], L1[0:64, :], True, True, ["W2A2", "L1"], [bkey])
            _act(P, sg[:], bk[:, :], AF.Sigmoid, [bkey, "vc"], ["sg"], bias=v(V_W0))
            bk, bkey = bank()
            _mm(P, bk[:, :], W2A2[64:128, :], L1[64:128, :], True, True, ["W2A2", "L1"], [bkey])
            _act(P, ai[:], bk[:, :], AF.Sigmoid, [bkey, "vc"], ["ai"], bias=v(V_A0))
            bk, bkey = bank()
            _mm(P, bk[:, :], G2[:, :], LG[:, :], True, True, ["G2", "LG"], [bkey])
            _cp(P, "act", gS[:], bk[:, :], [bkey], ["gS"])
            for i, (dst, dk, mc) in enumerate([(rm, "rm", V_MUR), (km, "km", V_MUK), (vm, "vm", V_MUV)]):
                rk_ = "R3_%d" % i
                _tt(P, "pool", dd[:], R3[i][:, 0:TT], R3[i][:, 1:TT + 1], ALU.subtract, [rk_], ["d4"])
                _stt(P, "dve", dst[:], dd[:], v(mc), R3[i][:, 1:TT + 1], ALU.mult, ALU.add, ["d4", "vc", rk_], [dk])
                _cp(P, "pool", R3[i][:, 0:1], R3[i][:, TT:TT + 1], [rk_], [rk_])
            _ts(P, "dve", kk[:], km[:], v(V_KK), None, ALU.mult, None, ["km", "vc"], ["kk"])
            _tt(P, "pool", kk2[:], kk[:], kk[:], ALU.mult, ["kk"], ["kk2"])
            bk, bkey = bank()
            _mm(P, bk[:, :], bones[:, :], kk2[:, :], True, True, ["bones", "kk2"], [bkey])
            _act(P, rn[:], bk[:, :], AF.Sqrt, [bkey], ["rn"], bias=1e-12)
            P.op("dve", lambda e: e.reciprocal(out=rn[:], in_=rn[:]), ["rn"], ["rn"])
            _tt(P, "dve", kkn[:], kk[:], rn[:], ALU.mult, ["kk", "rn"], ["kkn"])
            _ts(P, "dve", t1[:], ai[:], -1.0, v(V_KA), ALU.add, ALU.mult, ["ai", "vc"], ["t1"])
            _stt(P, "dve", kmod[:], t1[:], 1.0, km[:], ALU.add, ALU.mult, ["t1", "km"], ["kmod"])
            _ts(P, "pool", av[:], kkn[:], -1.0, None, ALU.mult, None, ["kkn"], ["av"])
            _tt(P, "pool", bv[:], kkn[:], ai[:], ALU.mult, ["kkn", "ai"], ["bv"])
            _ts(P, "dve", lw[:], sg[:], -float(np.exp(-0.5)), None, ALU.mult, None, ["sg"], ["lw"])
            P.op("dve", lambda e: e.tensor_tensor_scan(out=cc_[:], data0=rstm[:], data1=lw[:], initial=0.0,
                                                       op0=ALU.mult, op1=ALU.add), ["rstm", "lw"], ["cc"])
            _act(P, E1[:], cc_[:], AF.Exp, ["cc"], ["E1"])
            _act(P, E2[:], cc_[:], AF.Exp, ["cc"], ["E2"], scale=-1.0)
            _tt(P, "pool", d4[:], cc_[:], lw[:], ALU.subtract, ["cc", "lw"], ["d4"])
            _act(P, E3[:], d4[:], AF.Exp, ["d4"], ["E3"])
            _tt(P, "dve", r3(d4[:]), r3(cc_[:])[:, :, CH - 1:CH].to_broadcast([128, NCH, CH]), r3(cc_[:]),
                ALU.subtract, ["cc", "E3"], ["d4"])
            _act(P, E4[:], d4[:], AF.Exp, ["d4"], ["E4"])
            _cp(P, "dve", gC[:], r3(E1[:])[:, :, CH - 1], ["E1"], ["gC"])
            _tt(P, "dve", AR[:, :, 0, :], r3(av[:]), r3(E3[:]), ALU.mult, ["av", "E3"], ["AR"])
            _tt(P, "pool", AR[:, :, 1, :], r3(rm[:]), r3(E1[:]), ALU.mult, ["rm", "E1"], ["AR"])
            _tt(P, "dve", BK[:, :, 0, :], r3(bv[:]), r3(E2[:]), ALU.mult, ["bv", "E2"], ["BK"])
            _tt(P, "pool", BK[:, :, 1, :], r3(kmod[:]), r3(E2[:]), ALU.mult, ["kmod", "E2"], ["BK"])
            _tt(P, "dve", bh[:], bv[:], E4[:], ALU.mult, ["bv", "E4"], ["bh"])
            _tt(P, "pool", kh[:], kmod[:], E4[:], ALU.mult, ["kmod", "E4"], ["kh"])
            _cp(P, "act", vb[:], vm[:], ["vm"], ["vb"])
            for src, skey, dst, dkey in [(bh, "bh", Bt, "Bt"), (kh, "kh", Kt, "Kt"), (vb, "vb", Vt, "Vt")]:
                bk, bkey = bank()
                bkb = bk[:, :].bitcast(BF16)
                for j in range(NCH):
                    for h2 in range(2):
                        pr = slice(64 * h2, 64 * h2 + 64)
                        _tr(P, bkb[pr, j * CH:(j + 1) * CH], src[pr, j * CH:(j + 1) * CH], ident[pr, pr],
                            [skey, "ident"], [bkey], inc=(j == NCH - 1 and h2 == 1))
                _cp(P, "dve", dst[:], bkb[:, 0:TT].rearrange("p (j t) -> p j t", t=CH), [bkey], [dkey])
            for half in range(2):
                bkA, kA = bank()
                bkB, kB = bank()
                for jj in range(4):
                    j = half * 4 + jj
                    for h2 in range(2):
                        pr = slice(64 * h2, 64 * h2 + 64)
                        last = (jj == 3 and h2 == 1)
                        P.op("pe", lambda e, o=bkA[pr, jj * 128:(jj + 1) * 128], l=BK[pr, j, 0, :],
                             r=AR[pr, j, :, :].rearrange("p a t -> p (a t)"): e.matmul(o, l, r, start=True, stop=True),
                             ["BK", "AR"], [kA], inc=last)
                        P.op("pe", lambda e, o=bkB[pr, jj * 128:(jj + 1) * 128], l=BK[pr, j, 1, :],
                             r=AR[pr, j, :, :].rearrange("p a t -> p (a t)"): e.matmul(o, l, r, start=True, stop=True),
                             ["BK", "AR"], [kB], inc=last)
                js = slice(half * 4, half * 4 + 4)
                vA = bkA[:, :].rearrange("p (j a t) -> p j a t", j=4, a=2)
                vB = bkB[:, :].rearrange("p (j a t) -> p j a t", j=4, a=2)
                m_su = msk[:, 0, 0:256].rearrange("p (j t) -> p j t", t=CH)
                m_u = msk[:, 1, 0:256].rearrange("p (j t) -> p j t", t=CH)
                _tt(P, "dve", NTA[:, js, 0, :], vA[:, :, 0, :], m_su, ALU.mult, [kA, "msk"], ["NTA"])
                _tt(P, "dve", NTA[:, js, 1, :], vA[:, :, 1, :], m_u, ALU.mult, [kA, "msk"], ["NTA"])
                _tt(P, "dve", AKK[:, js, 0, :], vB[:, :, 0, :], m_su, ALU.mult, [kB, "msk"], ["AKK"])
                _tt(P, "dve", AKK[:, js, 1, :], vB[:, :, 1, :], m_u, ALU.mult, [kB, "msk"], ["AKK"])
            bkN, kN = bank()
            for j in range(NCH):
                for h2 in range(2):
                    pr = slice(64 * h2, 64 * h2 + 64)
                    P.op("pe", lambda e, o=bkN[pr, j * CH:(j + 1) * CH], l=AR[pr, j, 0, :], r=BK[pr, j, 0, :]:
                         e.matmul(o, l, r, start=True, stop=True), ["AR", "BK"], [kN], inc=(j == NCH - 1 and h2 == 1))
            idx = msk[:, 3, :].rearrange("p (j t) -> p j t", t=CH)
            mk = lambda i: msk[:, i, :].rearrange("p (j t) -> p j t", t=CH)
            _tt(P, "dve", Yf[:], r3(bkN[:, :]), mk(11), ALU.mult, [kN, "msk"], ["Yf"])
            _tt(P, "dve", Yf[:], Yf[:], idx, ALU.add, ["Yf", "msk"], ["Yf"])
            _tt(P, "pool", Xf[:], NTA[:, :, 0, :], mk(5), ALU.mult, ["NTA", "msk"], ["Xf"])
            _tt(P, "pool", Xf[:], Xf[:], idx, ALU.add, ["Xf", "msk"], ["Xf"])
            _cp(P, "act", Yb[:], Yf[:], ["Yf"], ["Yb"])
            _cp(P, "pool", Xb[:], Xf[:], ["Xf"], ["Xb"])

            def mm16(dstbank, dkey, L, lkey, Rr, rkey):
                for j in range(NCH):
                    for h2 in range(2):
                        pr = slice(64 * h2, 64 * h2 + 64)
                        P.op("pe", lambda e, o=dstbank[pr, j * CH:(j + 1) * CH], l=L[pr, j, :], r=Rr[pr, j, :]:
                             e.matmul(o, l, r, start=True, stop=True), [lkey, rkey], [dkey],
                             inc=(j == NCH - 1 and h2 == 1))
            NTl, Q1b = Ap[0], Ap[1]
            for lvl in range(1, 6):
                lastl = (lvl == 5)
                _tt(P, "pool", NTl[:], NTA[:, :, 0, :], mk(5 + lvl), ALU.mult, ["NTA", "msk"], ["Ap0"])
                b1, k1 = bank()
                mm16(b1, k1, NTl, "Ap0", Yb, "Yb")
                _cp(P, "act", Q1b[:], r3(b1[:, :]), [k1], ["Ap1"])
                if not lastl:
                    b2, k2 = bank()
                    mm16(b2, k2, Xb, "Xb", Q1b, "Ap1")
                b3, k3 = bank()
                mm16(b3, k3, Q1b, "Ap1", Xb, "Xb")
                if not lastl:
                    _tt(P, "dve", Yf[:], Yf[:], r3(b2[:, :]), ALU.add, ["Yf", k2], ["Yf"])
                    _cp(P, "act", Yb[:], Yf[:], ["Yf"], ["Yb"])
                _tt(P, "dve", Xf[:], Xf[:], r3(b3[:, :]), ALU.add, ["Xf", k3], ["Xf"])
                _cp(P, "pool", Xb[:], Xf[:], ["Xf"], ["Xb"])
            for j in range(NCH):
                for h2 in range(2):
                    pr = slice(64 * h2, 64 * h2 + 64)
                    _mm(P, pch[pr, 0:64], AR[pr, j, 0, :], Hb[pr, :], True, False, ["AR", "Hb"], ["pW"])
                    P.op("pe", lambda e, o=pch[pr, 0:64], l=AKK[pr, j, 0, :], r=Vt[pr, j, :]:
                         e.matmul(o, l, r, start=False, stop=True), ["AKK", "Vt"], ["pW"], inc=(h2 == 1))
                _cp(P, "dve", Wb[:], pch[:, 0:64], ["pW"], ["Wb"])
                for h2 in range(2):
                    pr = slice(64 * h2, 64 * h2 + 64)
                    P.op("pe", lambda e, o=pch[pr, 64:128], l=Xb[pr, j, :], r=Wb[pr, :]:
                         e.matmul(o, l, r, start=True, stop=True), ["Xb", "Wb"], ["pU"], inc=(h2 == 1))
                _cp(P, "dve", Ub[:], pch[:, 64:128], ["pU"], ["Ub"])
                for h2 in range(2):
                    pr = slice(64 * h2, 64 * h2 + 64)
                    oy = pyt[pr, j * CH:(j + 1) * CH]
                    _mm(P, oy, Hb[pr, :], AR[pr, j, 1, :], True, False, ["Hb", "AR"], ["pyt"])
                    _mm(P, oy, Ub[pr, :], NTA[pr, j, 1, :], False, False, ["Ub", "NTA"], ["pyt"])
                    P.op("pe", lambda e, o=oy, l=Vt[pr, j, :], r=AKK[pr, j, 1, :]:
                         e.matmul(o, l, r, start=False, stop=True), ["Vt", "AKK"], ["pyt"], inc=False)
                for h2 in range(2):
                    pr = slice(64 * h2, 64 * h2 + 64)
                    _mm(P, pch[pr, 128:192], Bt[pr, j, :], Ub[pr, :], True, False, ["Bt", "Ub"], ["pH"])
                    P.op("pe", lambda e, o=pch[pr, 128:192], l=Kt[pr, j, :], r=Vt[pr, j, :]:
                         e.matmul(o, l, r, start=False, stop=True), ["Kt", "Vt"], ["pH"], inc=(h2 == 1))
                _stt(P, "dve", H[:], H[:], gC[:, j:j + 1], pch[:, 128:192], ALU.mult, ALU.add, ["H", "gC", "pH"], ["H"])
                _cp(P, "act", Hb[:], H[:], ["H"], ["Hb"])
            _cp(P, "act", yT[:], pyt[:, :], ["pyt"], ["yT"])
            _cp(P, "pool", yTb[:], yT[:], ["yT"], ["yTb"])
            bk, bkey = bank()
            _mm(P, bk[:, :], bones[:, :], yTb[:, :], True, True, ["bones", "yTb"], [bkey])
            _stt(P, "dve", dY[:], bk[:, :], -1.0 / CH, yT[:], ALU.mult, ALU.add, [bkey, "yT"], ["dY"])
            _tt(P, "pool", d2b[:], dY[:], dY[:], ALU.mult, ["dY"], ["d2b"])
            bk, bkey = bank()
            _mm(P, bk[:, :], bones[:, :], d2b[:, :], True, True, ["bones", "d2b"], [bkey])
            _act(P, gn[:], bk[:, :], AF.Sqrt, [bkey], ["gn"], bias=GN_EPS, scale=1.0 / CH)
            P.op("dve", lambda e: e.reciprocal(out=gn[:], in_=gn[:]), ["gn"], ["gn"])
            _tt(P, "dve", gn[:], gn[:], dY[:], ALU.mult, ["gn", "dY"], ["gn"])
            _ts(P, "dve", gn[:], gn[:], v(V_LG), v(V_LB), ALU.mult, ALU.add, ["gn", "vc"], ["gn"])
            _stt(P, "dve", rkb[:], rm[:], v(V_RK), kmod[:], ALU.mult, ALU.mult, ["rm", "vc", "kmod"], ["rkb"])
            bk, bkey = bank()
            _mm(P, bk[:, :], bones[:, :], rkb[:, :], True, True, ["bones", "rkb"], [bkey])
            _tt(P, "dve", bon[:], bk[:, :], vm[:], ALU.mult, [bkey, "vm"], ["bon"])
            _tt(P, "pool", bon[:], bon[:], gn[:], ALU.add, ["bon", "gn"], ["bon"])
            _tt(P, "pool", yo[:], bon[:], gS[:], ALU.mult, ["bon", "gS"], ["yo"])
            out_toks.append(P.dma("sp", ya_o[:, t0:t0 + TT], yo[:], ["yo"], [], semkey="ya"))
            _ts(P, "dve", xc[:], XB[:, 3:TT + 3], v(V_CW3), v(V_CB), ALU.mult, ALU.add, ["XB", "vc"], ["kk"])
            for jx, col in [(2, V_CW2), (1, V_CW1), (0, V_CW0)]:
                _stt(P, "dve", xc[:], XB[:, jx:TT + jx], v(col), xc[:], ALU.mult, ALU.add, ["XB", "vc", "kk"], ["kk"])
            _cp(P, "pool", XB[:, 0:3], XB[:, TT:TT + 3], ["XB"], ["XB"])
            _cp(P, "act", xcb[:], xc[:], ["kk"], ["kk2"])
            for Wg, wk, dst, dk, bcol in [(Wrg, "Wrg", gr, "kkn", V_BR), (Wig, "Wig", gi, "t1", V_BI)]:
                bk, bkey = bank()
                for h2 in range(2):
                    pr = slice(64 * h2, 64 * h2 + 64)
                    P.op("pe", lambda e, o=bk[pr, :], l=Wg[pr, :], r=xcb[pr, :]: e.matmul(o, l, r, start=True, stop=True),
                         [wk, "kk2"], [bkey], inc=(h2 == 1))
                _act(P, dst[:], bk[:, :], AF.Sigmoid, [bkey, "vc"], [dk], bias=v(bcol))
            _act(P, aa[:], gr[:], AF.Exp, ["kkn", "m8"], ["E1"], scale=m8[:, 0:1])
            _act(P, a2[:], gr[:], AF.Exp, ["kkn", "m8"], ["E2"], scale=m8[:, 1:2])
            _act(P, sq[:], a2[:], AF.Sqrt, ["E2"], ["E3"], bias=1.0, scale=-1.0)
            _tt(P, "pool", uu[:], gi[:], xc[:], ALU.mult, ["t1", "kk"], ["E4"])
            _tt(P, "pool", uu[:], uu[:], sq[:], ALU.mult, ["E4", "E3"], ["E4"])
            P.op("dve", lambda e: e.tensor_tensor_scan(out=hh_[:], data0=aa[:], data1=uu[:], initial=hcar[:, 0:1],
                                                       op0=ALU.mult, op1=ALU.add), ["E1", "E4", "hcar"], ["d4"])
            _cp(P, "dve", hcar[:], hh_[:, TT - 1:TT], ["d4"], ["hcar"])
            _tt(P, "pool", ge[:], GB[:], GB[:], ALU.mult, ["GB"], ["lw"])
            _ts(P, "pool", ge[:], ge[:], 0.044715, 1.0, ALU.mult, ALU.add, ["lw"], ["lw"])
            _tt(P, "pool", ge[:], ge[:], GB[:], ALU.mult, ["lw", "GB"], ["lw"])
            _act(P, ge[:], ge[:], AF.Sigmoid, ["lw"], ["lw"], scale=1.5957691216057308)
            _tt(P, "pool", ge[:], ge[:], GB[:], ALU.mult, ["lw", "GB"], ["lw"])
            _tt(P, "dve", ybo[:], hh_[:], ge[:], ALU.mult, ["d4", "lw"], ["cc"])
            out_toks.append(P.dma("sp", yb_o[:, t0:t0 + TT], ybo[:], ["cc"], [], semkey="yb"))
        P.wait_all("sp", out_toks[-2:])
        P.emit()
    return nc


def _consts():
    import ml_dtypes
    p = np.arange(128)[:, None] % 64
    t = np.arange(512)[None, :] % 64
    z = np.zeros((128, 512), np.float32)
    msu = (p < t).astype(np.float32)
    mu_ = (p <= t).astype(np.float32)
    idx = (p == t).astype(np.float32)
    x = p ^ t
    hb = np.where(x > 0, np.floor(np.log2(np.maximum(x, 1))), -1).astype(np.int64)
    lv = [((p < t) & (hb == l)).astype(np.float32) for l in range(6)]
    m0l = ((p > t) & (hb == 0)).astype(np.float32)
    rst = np.ascontiguousarray(np.broadcast_to((t != 0).astype(np.float32), (128, 512)))
    cm = np.concatenate([msu, mu_, z, idx, z] + lv + [m0l], axis=1)
    bo = (np.arange(128)[:, None] // 64 == np.arange(128)[None, :] // 64).astype(np.float32)
    return np.eye(128, dtype=np.float32), bo, np.ascontiguousarray(cm), rst


def l1_inputs(inp, b, cs, seq):
    A = 512
    o1, o2, o3 = 3 * A, 3 * A + 512, 3 * A + 1024
    sl = slice(128 * cs, 128 * cs + 128)
    w_in = inp["w_in"][0]
    col = lambda a: np.ascontiguousarray(np.asarray(a, np.float32).reshape(-1)[sl])
    cid, cbo, cm, rst = _consts()
    vec = np.stack([col(inp["mu_rkv"][0][0:A]), col(inp["mu_rkv"][0][A:2 * A]), col(inp["mu_rkv"][0][2 * A:3 * A]),
                    col(inp["w0"][0]), col(inp["a0"][0]), col(inp["k_k"][0]), col(inp["k_a"][0]), col(inp["r_k"][0]),
                    col(inp["lnx_g"][0]), col(inp["lnx_b"][0]),
                    col(inp["conv_w"][0][0, 0]), col(inp["conv_w"][0][1, 0]), col(inp["conv_w"][0][2, 0]),
                    col(inp["conv_w"][0][3, 0]), col(inp["conv_b"][0]), col(inp["b_rgate"][0]), col(inp["b_igate"][0]),
                    col(inp["lam"][0])], axis=1)
    muT = np.concatenate([inp["mu_wag"][0][j].reshape(8, 128).T for j in range(3)], axis=1)
    return {
        "x": np.ascontiguousarray(inp["x"][b, :seq]),
        "cT": np.ascontiguousarray(inp["c"][b].reshape(8, 128).T),
        "wada": np.ascontiguousarray(inp["w_ada"][0][:, 0:2048]),
        "bada": np.ascontiguousarray(inp["b_ada"][0][None, 0:2048]),
        "gmix": np.ascontiguousarray(inp["g_mix"][0][None, :]),
        "wrkv": np.ascontiguousarray(np.concatenate([w_in[:, 128 * cs + A * i:128 * cs + A * i + 128] for i in range(3)], axis=1)),
        "wxg": np.ascontiguousarray(np.concatenate([w_in[:, o1 + 128 * cs:o1 + 128 * cs + 128],
                                                    w_in[:, o2 + 128 * cs:o2 + 128 * cs + 128]], axis=1)),
        "w1a1": np.ascontiguousarray(np.concatenate([inp["w1"][0], inp["a1"][0]], axis=1)),
        "g1": np.ascontiguousarray(inp["g1"][0]),
        "muT": np.ascontiguousarray(muT),
        "w2a2": np.ascontiguousarray(np.concatenate([inp["w2"][0][:, sl], inp["a2"][0][:, sl]], axis=0)),
        "g2o": np.ascontiguousarray(inp["g2"][0][:, sl]),
        "vecs": np.ascontiguousarray(vec.astype(np.float32)),
        "wrg": np.ascontiguousarray(inp["w_rgate"][0][2 * cs:2 * cs + 2].reshape(128, 64)),
        "wig": np.ascontiguousarray(inp["w_igate"][0][2 * cs:2 * cs + 2].reshape(128, 64)),
        "cid": cid, "cbo": cbo, "cmsk": cm, "crst": rst,
    }


def run_l1(inp, seq=SEQ):
    nc = build_l1(seq)
    maps = [l1_inputs(inp, c // 4, c % 4, seq) for c in range(8)]
    res = run_bass_kernel_spmd(nc, maps, core_ids=list(range(8)))
    ya = np.zeros((2, 512, seq), np.float32)
    yb = np.zeros((2, 512, seq), np.float32)
    for c in range(8):
        b, cs = c // 4, c % 4
        ya[b, 128 * cs:128 * cs + 128] = res.results[c]["ya"]
        yb[b, 128 * cs:128 * cs + 128] = res.results[c]["yb"]
    return ya, yb


NE = 32
FE = 256


def build_l2(ntok):
    nt = ntok // TT
    nc = bass.Bass("TRN2", target_bir_lowering=False)
    dr = lambda n, s, k="ExternalInput", dt=F32: nc.dram_tensor(n, list(s), dt, kind=k).ap()
    x = dr("x", [ntok, D])
    yaT = dr("yaT", [512, ntok])
    ybT = dr("ybT", [512, ntok])
    cT = dr("cT", [128, 8])
    wada = dr("wada", [D, 8192])
    bada = dr("bada", [1, 8192])
    gvec = dr("gvec", [3, D])
    wgate = dr("wgate", [D, 2048])
    pa = dr("pa", [512, D])
    pb = dr("pb", [512, D])
    wout = dr("wout", [D, D])
    wr = dr("wr", [D, 36])
    br = dr("br", [1, 36])
    w1e = dr("w1e", [NE, D, FE])
    w3e = dr("w3e", [NE, D, FE])
    w2e = dr("w2e", [NE, FE, D])
    cid = dr("cid", [128, 128])
    csel = dr("csel", [32, NE * 128])
    out = dr("out", [ntok, D], "ExternalOutput")

    es = ExitStack()
    with es:
        P = Prog(nc, es)
        sb, psum = P.sb, P.ps
        ident = sb("ident", [128, 128], BF16)
        sel = sb("sel", [32, NE, 128], BF16)
        Pa = sb("Pa", [128, 4, D], BF16)
        Pb = sb("Pb", [128, 4, D], BF16)
        Wo = sb("Wo", [128, 8, D], BF16)
        Wr = sb("Wr", [128, 8, 36], BF16)
        brb = sb("brb", [128, 36], F32)
        cTs = sb("cTs", [128, 8], F32)
        cact = sb("cact", [128, 8], F32)
        wad = sb("wad", [128, 8, 256], F32)
        badr = sb("badr", [1, 256], F32)
        ones1 = sb("ones1", [1, 128], F32)
        modb = sb("modb", [128, 8, D], F32)
        SH1, GS1, GT1, SH2, GS2, GT2, SHF, GSF = range(8)
        Wg = [sb("Wg%d" % i, [128, 8, 128], BF16) for i in range(2)]
        W1 = [sb("W1_%d" % i, [128, 8, FE], BF16) for i in range(2)]
        W3 = [sb("W3_%d" % i, [128, 8, FE], BF16) for i in range(2)]
        W2 = [sb("W2_%d" % i, [128, 2, D], BF16) for i in range(2)]
        xt = sb("xt", [128, 4, D], F32)
        junk = sb("junk", [128, D], F32)
        cbc = junk[:].rearrange("p (k c) -> p k c", k=8)
        ss = sb("ss", [128, 4], F32)
        rstd = sb("rstd", [128, 4], F32)
        hn = sb("hn", [128, 4, D], BF16)
        hT = sb("hT", [128, 8, TT], BF16)
        SG = sb("SG", [128, 16, TT], BF16)
        yat = sb("yat", [128, 4, TT], BF16)
        ybt = sb("ybt", [128, 4, TT], BF16)
        mT = sb("mT", [128, 8, TT], BF16)
        tA = sb("tA", [128, TT], F32)
        tB = sb("tB", [128, TT], F32)
        acc = sb("acc", [128, 4, D], F32)
        he = sb("he", [128, TT], F32)
        heb = [sb("heb%d" % i, [128, TT], BF16) for i in range(2)]
        lg = sb("lg", [128, 36], F32)
        sm = sb("sm", [128, 16], F32)
        mg = sb("mg", [128, 4], F32)
        les = sb("les", [128, 8], F32)
        le2 = sb("le2", [128, 8], F32)
        mk1 = sb("mk1", [128, 8], F32)
        mk2 = sb("mk2", [128, 8], F32)
        cw8 = sb("cw8", [128, 8], F32)
        cwb = sb("cwb", [128, 32], BF16)
        cwT = sb("cwT", [32, TT], BF16)

        pool_banks = [psum("pb%d" % i, [128, 512], F32) for i in range(8)]

        def bank():
            i = P.nps % 8
            P.nps += 1
            return pool_banks[i], ("pb", i)

        ld = lambda o, i, key, eng="sp": P.dma(eng, o, i, [], [key], semkey=key)
        ld(ident[:], cid[:, :], "ident", "pool")
        ld(sel[:], csel.rearrange("r (e t) -> r e t", e=NE), "sel", "pool")
        ld(Pa[:], pa.rearrange("(k p) c -> p k c", p=128), "Pa", "pool")
        ld(Pb[:], pb.rearrange("(k p) c -> p k c", p=128), "Pb", "pool")
        ld(Wo[:], wout.rearrange("(k p) c -> p k c", p=128), "Wo", "pool")
        ld(Wr[:], wr.rearrange("(k p) c -> p k c", p=128), "Wr", "pool")
        ld(brb[:], br.partition_broadcast(128), "brb")
        ld(cTs[:], cT[:, :], "cTs")
        P.op("dve", lambda e: e.memset(ones1[:], 1.0), [], ["ones1"])
        _act(P, cact[:], cTs[:], AF.Silu, ["cTs"], ["cact"])
        for k in range(8):
            _cp(P, "dve", cbc[:, k, :], cact[:, k:k + 1].to_broadcast([128, 128]), ["cact"], ["junk"])
        modf = modb[:].rearrange("p a d -> p (a d)")
        for cc in range(32):
            ld(wad[:], wada[:, cc * 256:(cc + 1) * 256].rearrange("(k p) c -> p k c", p=128), "wad")
            ld(badr[:], bada[:, cc * 256:(cc + 1) * 256], "badr")
            bk, bkey = bank()
            for k in range(8):
                _mm(P, bk[:, 0:256], cbc[:, k, :], wad[:, k, :], k == 0, False, ["junk", "wad"], [bkey])
            _mm(P, bk[:, 0:256], ones1[:, :], badr[:, :], False, True, ["ones1", "badr"], [bkey])
            _cp(P, "dve", modf[:, cc * 256:(cc + 1) * 256], bk[:, 0:256], [bkey], ["modb"])
        for gi_, slot in [(0, GS1), (1, GS2), (2, GSF)]:
            ld(junk[:], gvec[gi_:gi_ + 1, :].partition_broadcast(128), "junk")
            _stt(P, "dve", modb[:, slot, :], modb[:, slot, :], 1.0, junk[:], ALU.add, ALU.mult, ["modb", "junk"], ["modb"])

        def norm_mod(src_key, gslot, sslot):
            for s in range(4):
                _act(P, junk[:], xt[:, s, :], AF.Square, ["xt"], ["junk", "ss"], accum=ss[:, s:s + 1])
            _act(P, rstd[:], ss[:], AF.Sqrt, ["ss"], ["rstd"], bias=EPS, scale=1.0 / D)
            P.op("dve", lambda e: e.reciprocal(out=rstd[:], in_=rstd[:]), ["rstd"], ["rstd"])
            for s in range(4):
                _stt(P, "dve", junk[:], xt[:, s, :], rstd[:, s:s + 1], modb[:, gslot, :], ALU.mult, ALU.mult,
                     ["xt", "rstd", "modb"], ["junk"])
                _tt(P, "pool", hn[:, s, :], junk[:], modb[:, sslot, :], ALU.add, ["junk", "modb"], ["hn"])
            for s in range(4):
                bk, bkey = bank()
                bkb = bk[:, :].bitcast(BF16)
                for k in range(8):
                    _tr(P, bkb[:, k * 128:(k + 1) * 128], hn[:, s, k * 128:(k + 1) * 128], ident[:],
                        ["hn", "ident"], [bkey], inc=(k == 7))
                _cp(P, "act" if s % 2 else "dve", hT[:, :, 128 * s:128 * (s + 1)],
                    bkb[:, 0:1024].rearrange("p (k t) -> p k t", k=8), [bkey], ["hT"])

        out_toks = []
        wcnt = 0
        for it in range(nt):
            t0 = it * TT
            P.dma("sp", xt[:], x[t0:t0 + TT, :].rearrange("(s p) d -> p s d", p=128), [], ["xt"], semkey="xt")
            P.dma("pool", yat[:], yaT[:, t0:t0 + TT].rearrange("(k p) t -> p k t", p=128), [], ["yat"], semkey="yat")
            P.dma("pool", ybt[:], ybT[:, t0:t0 + TT].rearrange("(k p) t -> p k t", p=128), [], ["ybt"], semkey="ybt")
            norm_mod("xt", GS1, SH1)
            for c in range(16):
                sl_ = c % 2
                P.dma("pool", Wg[sl_][:], wgate[:, c * 128:(c + 1) * 128].rearrange("(k p) c -> p k c", p=128),
                      [], ["Wg%d" % sl_], semkey="Wg%d" % sl_)
                bk, bkey = bank()
                for k in range(8):
                    _mm(P, bk[:, :], Wg[sl_][:, k, :], hT[:, k, :], k == 0, k == 7, ["Wg%d" % sl_, "hT"], [bkey])
                _act(P, SG[:, c, :], bk[:, :], AF.Sigmoid, [bkey], ["SG"])
            for dc in range(8):
                bA, kA = bank()
                for k in range(4):
                    _mm(P, bA[:, :], Pa[:, k, dc * 128:(dc + 1) * 128], yat[:, k, :], k == 0, k == 3, ["Pa", "yat"], [kA])
                bB, kB = bank()
                for k in range(4):
                    _mm(P, bB[:, :], Pb[:, k, dc * 128:(dc + 1) * 128], ybt[:, k, :], k == 0, k == 3, ["Pb", "ybt"], [kB])
                _tt(P, "dve", tA[:], bA[:, :], SG[:, dc, :], ALU.mult, [kA, "SG"], ["tA"])
                _tt(P, "dve", tB[:], bB[:, :], SG[:, 8 + dc, :], ALU.mult, [kB, "SG"], ["tB"])
                _tt(P, "pool", mT[:, dc, :], tA[:], tB[:], ALU.add, ["tA", "tB"], ["mT"])
            for s in range(4):
                for dh in range(2):
                    bk, bkey = bank()
                    for k in range(8):
                        _mm(P, bk[:, :], mT[:, k, s * 128:(s + 1) * 128], Wo[:, k, dh * 512:(dh + 1) * 512], k == 0, k == 7,
                            ["mT", "Wo"], [bkey])
                    _tt(P, "dve", junk[:, 0:512], bk[:, :], modb[:, GT1, dh * 512:(dh + 1) * 512], ALU.mult,
                        [bkey, "modb"], ["junk"])
                    _tt(P, "pool", xt[:, s, dh * 512:(dh + 1) * 512], xt[:, s, dh * 512:(dh + 1) * 512], junk[:, 0:512],
                        ALU.add, ["xt", "junk"], ["xt"])
            norm_mod("xt", GS2, SH2)
            for s in range(4):
                bk, bkey = bank()
                for k in range(8):
                    _mm(P, bk[:, 0:36], hT[:, k, s * 128:(s + 1) * 128], Wr[:, k, :], k == 0, k == 7, ["hT", "Wr"], [bkey])
                _tt(P, "dve", lg[:], bk[:, 0:36], brb[:], ALU.add, [bkey, "brb"], ["lg"])
                R_, W_ = ["lg", "sm", "mg", "les", "le2", "mk1", "mk2", "cw8"], None
                dv = lambda fn, rd, wr_: P.op("dve", fn, rd, wr_)
                dv(lambda e: e.reduce_max(out=sm[:, 0:1], in_=lg[:, 0:4], axis=AX.X), ["lg"], ["sm"])
                _ts(P, "dve", mg[:], lg[:, 0:4], sm[:, 0:1], None, ALU.is_equal, None, ["lg", "sm"], ["mg"])
                _ts(P, "dve", sm[:, 1:2], sm[:, 0:1], -1.0, None, ALU.mult, None, ["sm"], ["sm"])
                _act(P, le2[:, 0:4], lg[:, 0:4], AF.Exp, ["lg", "sm"], ["le2", "sm"], bias=sm[:, 1:2], accum=sm[:, 2:3])
                dv(lambda e: e.reciprocal(out=sm[:, 3:4], in_=sm[:, 2:3]), ["sm"], ["sm"])
                _ts(P, "dve", les[:], lg[:, 4:12], mg[:, 0:1], None, ALU.mult, None, ["lg", "mg"], ["les"])
                for g_ in range(1, 4):
                    _stt(P, "dve", les[:], lg[:, 4 + 8 * g_:12 + 8 * g_], mg[:, g_:g_ + 1], les[:], ALU.mult, ALU.add,
                         ["lg", "mg", "les"], ["les"])
                dv(lambda e: e.reduce_max(out=sm[:, 4:5], in_=les[:], axis=AX.X), ["les"], ["sm"])
                _ts(P, "dve", mk1[:], les[:], sm[:, 4:5], None, ALU.is_equal, None, ["les", "sm"], ["mk1"])
                _stt(P, "dve", le2[:], mk1[:], -1e30, les[:], ALU.mult, ALU.add, ["mk1", "les"], ["le2"])
                dv(lambda e: e.reduce_max(out=sm[:, 5:6], in_=le2[:], axis=AX.X), ["le2"], ["sm"])
                _ts(P, "dve", mk2[:], le2[:], sm[:, 5:6], None, ALU.is_equal, None, ["le2", "sm"], ["mk2"])
                _tt(P, "dve", sm[:, 6:7], sm[:, 5:6], sm[:, 4:5], ALU.subtract, ["sm"], ["sm"])
                _act(P, sm[:, 7:8], sm[:, 6:7], AF.Exp, ["sm"], ["sm"])
                _ts(P, "dve", sm[:, 8:9], sm[:, 7:8], 1.0, None, ALU.add, None, ["sm"], ["sm"])
                dv(lambda e: e.reciprocal(out=sm[:, 9:10], in_=sm[:, 8:9]), ["sm"], ["sm"])
                _tt(P, "dve", sm[:, 10:11], sm[:, 9:10], sm[:, 7:8], ALU.mult, ["sm"], ["sm"])
                _tt(P, "dve", sm[:, 11:12], sm[:, 9:10], sm[:, 3:4], ALU.mult, ["sm"], ["sm"])
                _tt(P, "dve", sm[:, 12:13], sm[:, 10:11], sm[:, 3:4], ALU.mult, ["sm"], ["sm"])
                _ts(P, "dve", cw8[:], mk1[:], sm[:, 11:12], None, ALU.mult, None, ["mk1", "sm"], ["cw8"])
                _stt(P, "dve", cw8[:], mk2[:], sm[:, 12:13], cw8[:], ALU.mult, ALU.add, ["mk2", "sm", "cw8"], ["cw8"])
                for g_ in range(4):
                    _ts(P, "dve", cwb[:, 8 * g_:8 * g_ + 8], cw8[:], mg[:, g_:g_ + 1], None, ALU.mult, None,
                        ["cw8", "mg"], ["cwb"])
                bk, bkey = bank()
                bkb = bk[:, :].bitcast(BF16)
                _tr(P, bkb[0:32, 0:128], cwb[:, :], ident[:], ["cwb", "ident"], [bkey])
                _cp(P, "dve", cwT[:, s * 128:(s + 1) * 128], bkb[0:32, 0:128], [bkey], ["cwT"])
            for e_ in range(NE):
                sl_ = wcnt % 2
                wcnt += 1
                k1, k3, k2 = "W1_%d" % sl_, "W3_%d" % sl_, "W2_%d" % sl_
                P.dma("pool", W1[sl_][:], w1e[e_].rearrange("(k p) f -> p k f", p=128), [], [k1], semkey=k1)
                P.dma("pool", W3[sl_][:], w3e[e_].rearrange("(k p) f -> p k f", p=128), [], [k3], semkey=k3)
                P.dma("pool", W2[sl_][:], w2e[e_].rearrange("(f p) d -> p f d", p=128), [], [k2], semkey=k2)
                bC, kC = bank()
                _mm(P, bC[:, :], sel[:, e_, :], cwT[:, :], True, True, ["sel", "cwT"], [kC])
                for f in range(2):
                    b1, kb1 = bank()
                    for k in range(8):
                        _mm(P, b1[:, :], W1[sl_][:, k, f * 128:(f + 1) * 128], hT[:, k, :], k == 0, k == 7, [k1, "hT"], [kb1])
                    b3, kb3 = bank()
                    for k in range(8):
                        _mm(P, b3[:, :], W3[sl_][:, k, f * 128:(f + 1) * 128], hT[:, k, :], k == 0, k == 7, [k3, "hT"], [kb3])
                    _act(P, he[:], b1[:, :], AF.Silu, [kb1], ["he"])
                    _tt(P, "dve", he[:], he[:], b3[:, :], ALU.mult, ["he", kb3], ["he"])
                    _tt(P, "dve", heb[f][:], he[:], bC[:, :], ALU.mult, ["he", kC], ["heb%d" % f])
                for s in range(4):
                    for dh in range(2):
                        bk, bkey = bank()
                        for f in range(2):
                            _mm(P, bk[:, :], heb[f][:, s * 128:(s + 1) * 128], W2[sl_][:, f, dh * 512:(dh + 1) * 512],
                                f == 0, f == 1, ["heb%d" % f, k2], [bkey])
                        dst = acc[:, s, dh * 512:(dh + 1) * 512]
                        if e_ == 0:
                            _cp(P, "dve", dst, bk[:, :], [bkey], ["acc"])
                        else:
                            _tt(P, "dve", dst, dst, bk[:, :], ALU.add, ["acc", bkey], ["acc"])
            for s in range(4):
                _tt(P, "pool", acc[:, s, :], acc[:, s, :], modb[:, GT2, :], ALU.mult, ["acc", "modb"], ["acc"])
                _tt(P, "pool", xt[:, s, :], xt[:, s, :], acc[:, s, :], ALU.add, ["xt", "acc"], ["xt"])
            for s in range(4):
                _act(P, junk[:], xt[:, s, :], AF.Square, ["xt"], ["junk", "ss"], accum=ss[:, s:s + 1])
            _act(P, rstd[:], ss[:], AF.Sqrt, ["ss"], ["rstd"], bias=EPS, scale=1.0 / D)
            P.op("dve", lambda e: e.reciprocal(out=rstd[:], in_=rstd[:]), ["rstd"], ["rstd"])
            for s in range(4):
                _stt(P, "dve", acc[:, s, :], xt[:, s, :], rstd[:, s:s + 1], modb[:, GSF, :], ALU.mult, ALU.mult,
                     ["xt", "rstd", "modb"], ["acc"])
                _tt(P, "pool", acc[:, s, :], acc[:, s, :], modb[:, SHF, :], ALU.add, ["acc", "modb"], ["acc"])
            out_toks.append(P.dma("sp", out[t0:t0 + TT, :].rearrange("(s p) d -> p s d", p=128), acc[:], ["acc"], [],
                                  semkey="out"))
        P.wait_all("sp", out_toks[-1:])
        P.emit()
    return nc


def l2_inputs(inp, ya, yb, b, sg, ntok):
    t0 = sg * ntok
    o3 = 3 * 512 + 1024
    selm = np.zeros((32, NE, 128), np.float32)
    for e in range(NE):
        selm[e, e, :] = 1.0
    return {
        "x": np.ascontiguousarray(inp["x"][b, t0:t0 + ntok]),
        "yaT": np.ascontiguousarray(ya[b][:, t0:t0 + ntok]),
        "ybT": np.ascontiguousarray(yb[b][:, t0:t0 + ntok]),
        "cT": np.ascontiguousarray(inp["c"][b].reshape(8, 128).T),
        "wada": np.ascontiguousarray(np.concatenate([inp["w_ada"][0], inp["w_ada_f"]], axis=1)),
        "bada": np.ascontiguousarray(np.concatenate([inp["b_ada"][0], inp["b_ada_f"]])[None, :]),
        "gvec": np.ascontiguousarray(np.stack([inp["g_mix"][0], inp["g_ffn"][0], inp["g_final"]])),
        "wgate": np.ascontiguousarray(inp["w_in"][0][:, o3:]),
        "pa": np.ascontiguousarray(inp["p_a"][0]), "pb": np.ascontiguousarray(inp["p_b"][0]),
        "wout": np.ascontiguousarray(inp["w_out"][0]),
        "wr": np.ascontiguousarray(np.concatenate([inp["w_rg"][0], inp["w_re"][0]], axis=1)),
        "br": np.ascontiguousarray(np.concatenate([inp["b_rg"][0], inp["b_re"][0]])[None, :]),
        "w1e": np.ascontiguousarray(inp["w1e"][0]), "w3e": np.ascontiguousarray(inp["w3e"][0]),
        "w2e": np.ascontiguousarray(inp["w2e"][0]),
        "cid": np.eye(128, dtype=np.float32), "csel": selm.reshape(32, NE * 128),
    }


def run_l2(inp, ya, yb, seq=SEQ):
    ntok = seq // 4
    nc = build_l2(ntok)
    maps = [l2_inputs(inp, ya, yb, c // 4, c % 4, ntok) for c in range(8)]
    res = run_bass_kernel_spmd(nc, maps, core_ids=list(range(8)))
    out = np.zeros((2, seq, D), np.float32)
    for c in range(8):
        out[c // 4, (c % 4) * ntok:(c % 4 + 1) * ntok] = res.results[c]["out"]
    return out


def kernel(**inputs):
    inp = {k: np.asarray(v) for k, v in inputs.items()}
    ya, yb = run_l1(inp, SEQ)
    return run_l2(inp, ya, yb, SEQ)
```
